# Optimizing a Trainium2 kernel written in Bass

```python
import math
import jax
import jax.numpy as jnp
from jax import lax
import numpy as np

D_MODEL = 2048
BATCH = 4
SEQ = 2048
DEPTH = 4

CHUNK = 64
MEM_LEN = 256
Q_BLOCK = 128
N_EVEN = (DEPTH + 1) // 2
N_ODD = DEPTH // 2

GMLP_CHUNK = 128
A_WIDTH = D_MODEL // 2
A_HEADS = 8
A_HEAD_DIM = A_WIDTH // A_HEADS
B_WIDTH = D_MODEL // 2
POOL_WINDOWS = (2, 4, 8, 16)
N_POOL = len(POOL_WINDOWS)
B_GROUP = B_WIDTH // N_POOL
MIX_IN = 2 * A_WIDTH + B_WIDTH
C_HEADS = 16
C_HEAD_DIM = D_MODEL // (2 * C_HEADS)
C_V_DIM = 2 * C_HEAD_DIM
REL_BUCKETS = 32
REL_MAX_DIST = 128
X_HEADS = 4
X_HEAD_DIM = D_MODEL // X_HEADS
D_FF = ((8 * D_MODEL) // 3 + 255) // 256 * 256
N_EXPERTS = 8
TOP_K = 2
D_FF_EXPERT = D_FF // 2
EXPERT_BLOCK = 256
DN_ALPHA = (2 * DEPTH) ** 0.25
DN_BETA = (8 * DEPTH) ** -0.25
LN_EPS = 1e-5
NEG = -1e30

kernel_name = 'hybrid_gmlp_pool_diffattn_moe_trunk'


def layer_norm(x, g, b):
    xf = x.astype(jnp.float32)
    mu = jnp.mean(xf, -1, keepdims=True)
    var = jnp.mean(jnp.square(xf - mu), -1, keepdims=True)
    y = (xf - mu) * lax.rsqrt(var + LN_EPS)
    return (y * g.astype(jnp.float32) + b.astype(jnp.float32)).astype(x.dtype)


def rms_norm(x, g):
    xf = x.astype(jnp.float32)
    y = xf * lax.rsqrt(jnp.mean(xf * xf, -1, keepdims=True) + LN_EPS)
    return (y * g.astype(jnp.float32)).astype(x.dtype)


def t5_bucket(rel):
    half = REL_BUCKETS // 2
    max_exact = half // 2
    ret = jnp.where(rel > 0, half, 0)
    n = jnp.abs(rel)
    nf = jnp.maximum(n, 1).astype(jnp.float32)
    large = max_exact + (jnp.log(nf / max_exact) / math.log(REL_MAX_DIST / max_exact) * (half - max_exact)).astype(jnp.int32)
    large = jnp.minimum(large, half - 1)
    return ret + jnp.where(n < max_exact, n, large)


def gmlp_pool_mixer(x, w_in, ln_v_g, ln_v_b, w_s, b_s, w_pool, pool_scale, w_out):
    bsz, S, _ = x.shape
    h = x @ w_in
    z = jax.nn.gelu(h[..., :2 * A_WIDTH])
    u, v = z[..., :A_WIDTH], z[..., A_WIDTH:]
    v = layer_norm(v, ln_v_g, ln_v_b)
    nc = S // GMLP_CHUNK
    vc = v.reshape(bsz, nc, GMLP_CHUNK, A_HEADS, A_HEAD_DIM)
    pos = jnp.arange(GMLP_CHUNK)
    allowed = (pos[None, :] // CHUNK) <= (pos[:, None] // CHUNK)
    ws = jnp.where(allowed[None], w_s, jnp.zeros((), w_s.dtype))
    sv = jnp.einsum('hpq,bnqhc->bnphc', ws, vc) + b_s.T[:, :, None]
    a_out = u * sv.reshape(bsz, S, A_WIDTH)
    pin = h[..., 2 * A_WIDTH:].reshape(bsz, S, N_POOL, B_GROUP)
    cs = jnp.cumsum(pin.astype(jnp.float32), axis=1)
    t1 = jnp.arange(1, S + 1)
    pooled = []
    for gi, w in enumerate(POOL_WINDOWS):
        c = cs[:, :, gi]
        lag = jnp.pad(c[:, :S - w], ((0, 0), (w, 0), (0, 0)))
        cnt = jnp.minimum(t1, w).astype(jnp.float32)[None, :, None]
        pooled.append((c - lag) / cnt)
    pooled = jnp.stack(pooled, axis=2).astype(x.dtype) - pin
    b_out = jnp.einsum('bsgc,gcd->bsgd', pooled, w_pool).reshape(bsz, S, B_WIDTH) * pool_scale
    return jnp.concatenate([a_out, b_out], axis=-1) @ w_out


def diff_attention(x, w_qkv, lam_q1, lam_k1, lam_q2, lam_k2, subln_g, w_o, rel_table, lam_init):
    bsz, S, _ = x.shape
    qkv = x @ w_qkv
    q = qkv[..., :D_MODEL].reshape(bsz, S, C_HEADS, 2, C_HEAD_DIM)
    k = qkv[..., D_MODEL:2 * D_MODEL].reshape(bsz, S, C_HEADS, 2, C_HEAD_DIM)
    v = qkv[..., 2 * D_MODEL:].reshape(bsz, S, C_HEADS, C_V_DIM)
    lam = (jnp.exp(jnp.sum(lam_q1 * lam_k1).astype(jnp.float32))
           - jnp.exp(jnp.sum(lam_q2 * lam_k2).astype(jnp.float32)) + lam_init)
    scale = C_HEAD_DIM ** -0.5
    outs = []
    for qb in range(S // Q_BLOCK):
        q0 = qb * Q_BLOCK
        kend = q0 + Q_BLOCK
        qi = jnp.arange(q0, kend)
        kj = jnp.arange(kend)
        bias = jnp.transpose(rel_table[t5_bucket(kj[None, :] - qi[:, None])], (2, 0, 1)).astype(jnp.float32)
        allowed = (kj[None, :] // CHUNK) <= (qi[:, None] // CHUNK)
        s = jnp.einsum('bqhmd,bkhmd->bmhqk', q[:, q0:kend], k[:, :kend]).astype(jnp.float32) * scale + bias
        s = jnp.where(allowed, s, NEG)
        p = jax.nn.softmax(s, axis=-1)
        p = p[:, 0] - lam * p[:, 1]
        outs.append(jnp.einsum('bhqk,bkhe->bqhe', p.astype(v.dtype), v[:, :kend]))
    o = jnp.concatenate(outs, axis=1)
    o = rms_norm(o, subln_g) * (1.0 - lam_init)
    return o.reshape(bsz, S, D_MODEL) @ w_o


def memory_cross_attention(x, mem, w_q, w_kv, w_o):
    bsz, S, _ = x.shape
    q = (x @ w_q).reshape(bsz, S, X_HEADS, X_HEAD_DIM)
    kv = mem @ w_kv
    k = kv[..., :D_MODEL].reshape(bsz, -1, X_HEADS, X_HEAD_DIM)
    v = kv[..., D_MODEL:].reshape(bsz, -1, X_HEADS, X_HEAD_DIM)
    s = jnp.einsum('bqhd,bkhd->bhqk', q, k).astype(jnp.float32) * (X_HEAD_DIM ** -0.5)
    p = jax.nn.softmax(s, axis=-1)
    o = jnp.einsum('bhqk,bkhd->bqhd', p.astype(v.dtype), v)
    return o.reshape(bsz, S, D_MODEL) @ w_o


def swiglu(x, w_gate, w_up, w_down):
    return (jax.nn.silu(x @ w_gate) * (x @ w_up)) @ w_down


def moe_swiglu(x, w_router, w_gate, w_up, w_down):
    bsz, S, D = x.shape
    xf = x.reshape(-1, D)
    N = xf.shape[0]
    logits = (xf @ w_router).astype(jnp.float32)
    top_logits, top_idx = lax.top_k(logits, TOP_K)
    top_w = jax.nn.softmax(top_logits, axis=-1)
    M = N * TOP_K
    flat_e = top_idx.reshape(-1)
    flat_tok = jnp.repeat(jnp.arange(N, dtype=jnp.int32), TOP_K)
    flat_w = top_w.reshape(-1)
    order = jnp.argsort(flat_e)
    sorted_e = flat_e[order]
    counts = jnp.bincount(flat_e, length=N_EXPERTS)
    starts = jnp.cumsum(counts) - counts
    padded = (counts + EXPERT_BLOCK - 1) // EXPERT_BLOCK * EXPERT_BLOCK
    pad_ends = jnp.cumsum(padded)
    pad_starts = pad_ends - padded
    dest = pad_starts[sorted_e] + jnp.arange(M, dtype=jnp.int32) - starts[sorted_e]
    n_blocks = (M + N_EXPERTS * (EXPERT_BLOCK - 1) + EXPERT_BLOCK - 1) // EXPERT_BLOCK
    P = n_blocks * EXPERT_BLOCK
    buf_tok = jnp.full((P,), N, jnp.int32).at[dest].set(flat_tok[order])
    buf_w = jnp.zeros((P,), jnp.float32).at[dest].set(flat_w[order])
    block_start = jnp.arange(n_blocks, dtype=jnp.int32) * EXPERT_BLOCK
    block_e = jnp.minimum(jnp.searchsorted(pad_ends, block_start, side='right'), N_EXPERTS - 1)
    x_pad = jnp.concatenate([xf, jnp.zeros((1, D), xf.dtype)], axis=0)
    xb = x_pad[buf_tok].reshape(n_blocks, EXPERT_BLOCK, D)

    def expert_block(args):
        xblk, e = args
        return swiglu(xblk, w_gate[e], w_up[e], w_down[e])

    yb = lax.map(expert_block, (xb, block_e)).reshape(P, D)
    out = jnp.zeros((N + 1, D), jnp.float32).at[buf_tok].add(yb.astype(jnp.float32) * buf_w[:, None])[:N]
    return out.astype(x.dtype).reshape(bsz, S, D)


def setup_inputs(seed: int = 0) -> dict:
    key = jax.random.key(seed)
    ks = iter(jax.random.split(key, 32))

    def nrm(shape, scale):
        return jax.random.normal(next(ks), shape, jnp.float32) * scale

    D = D_MODEL
    ne, no = N_EVEN, N_ODD
    return {
        'x': nrm((BATCH, SEQ, D), 1.0),
        'mem': nrm((BATCH, MEM_LEN, D), 1.0),
        'rel_table': nrm((REL_BUCKETS, C_HEADS), 0.5),
        'mix_w_in': nrm((ne, D, MIX_IN), D ** -0.5),
        'gmlp_ln_g': 1.0 + nrm((ne, A_WIDTH), 0.02),
        'gmlp_ln_b': nrm((ne, A_WIDTH), 0.02),
        'gmlp_w_s': nrm((ne, A_HEADS, GMLP_CHUNK, GMLP_CHUNK), GMLP_CHUNK ** -0.5),
        'gmlp_b_s': 1.0 + nrm((ne, A_HEADS, GMLP_CHUNK), 0.02),
        'pool_w': nrm((ne, N_POOL, B_GROUP, B_GROUP), B_GROUP ** -0.5),
        'pool_scale': 1.0 + nrm((ne, B_WIDTH), 0.02),
        'mix_w_out': nrm((ne, D, D), D ** -0.5 * DN_BETA),
        'diff_w_qkv': nrm((no, D, 3 * D), D ** -0.5),
        'diff_lam_q1': nrm((no, C_HEAD_DIM), 0.1),
        'diff_lam_k1': nrm((no, C_HEAD_DIM), 0.1),
        'diff_lam_q2': nrm((no, C_HEAD_DIM), 0.1),
        'diff_lam_k2': nrm((no, C_HEAD_DIM), 0.1),
        'diff_subln_g': 1.0 + nrm((no, C_V_DIM), 0.02),
        'diff_w_o': nrm((no, D, D), D ** -0.5 * DN_BETA),
        'xa_w_q': nrm((DEPTH, D, D), D ** -0.5),
        'xa_w_kv': nrm((DEPTH, D, 2 * D), D ** -0.5),
        'xa_w_o': nrm((DEPTH, D, D), D ** -0.5 * DN_BETA),
        'ffn_w_gate': nrm((ne, D, D_FF), D ** -0.5),
        'ffn_w_up': nrm((ne, D, D_FF), D ** -0.5),
        'ffn_w_down': nrm((ne, D_FF, D), D_FF ** -0.5 * DN_BETA),
        'moe_w_router': nrm((no, D, N_EXPERTS), D ** -0.5),
        'moe_w_gate': nrm((no, N_EXPERTS, D, D_FF_EXPERT), D ** -0.5),
        'moe_w_up': nrm((no, N_EXPERTS, D, D_FF_EXPERT), D ** -0.5),
        'moe_w_down': nrm((no, N_EXPERTS, D_FF_EXPERT, D), D_FF_EXPERT ** -0.5 * DN_BETA),
        'ln_g': 1.0 + nrm((DEPTH, 3, D), 0.02),
        'ln_b': nrm((DEPTH, 3, D), 0.02),
    }


def reference(x, mem, rel_table, mix_w_in, gmlp_ln_g, gmlp_ln_b, gmlp_w_s, gmlp_b_s, pool_w, pool_scale,
              mix_w_out, diff_w_qkv, diff_lam_q1, diff_lam_k1, diff_lam_q2, diff_lam_k2, diff_subln_g, diff_w_o,
              xa_w_q, xa_w_kv, xa_w_o, ffn_w_gate, ffn_w_up, ffn_w_down, moe_w_router, moe_w_gate, moe_w_up,
              moe_w_down, ln_g, ln_b):
    h = x
    for layer in range(DEPTH):
        i = layer // 2
        if layer % 2 == 0:
            mix = gmlp_pool_mixer(h, mix_w_in[i], gmlp_ln_g[i], gmlp_ln_b[i], gmlp_w_s[i], gmlp_b_s[i],
                                  pool_w[i], pool_scale[i], mix_w_out[i])
        else:
            lam_init = 0.8 - 0.6 * math.exp(-0.3 * layer)
            mix = diff_attention(h, diff_w_qkv[i], diff_lam_q1[i], diff_lam_k1[i], diff_lam_q2[i], diff_lam_k2[i],
                                 diff_subln_g[i], diff_w_o[i], rel_table, lam_init)
        h = layer_norm(DN_ALPHA * h + mix, ln_g[layer, 0], ln_b[layer, 0])
        xa = memory_cross_attention(h, mem, xa_w_q[layer], xa_w_kv[layer], xa_w_o[layer])
        h = layer_norm(DN_ALPHA * h + xa, ln_g[layer, 1], ln_b[layer, 1])
        if layer % 2 == 0:
            ff = swiglu(h, ffn_w_gate[i], ffn_w_up[i], ffn_w_down[i])
        else:
            ff = moe_swiglu(h, moe_w_router[i], moe_w_gate[i], moe_w_up[i], moe_w_down[i])
        h = layer_norm(DN_ALPHA * h + ff, ln_g[layer, 2], ln_b[layer, 2])
    return h
```

```python
import math
from contextlib import ExitStack

import numpy as np
import concourse.bass as bass
import concourse.mybir as mybir
from concourse.bass_utils import run_bass_kernel_spmd

F32 = mybir.dt.float32
BF16 = mybir.dt.bfloat16
AF = mybir.ActivationFunctionType
ALU = mybir.AluOpType
AX = mybir.AxisListType

D = 2048
T = 1024
NT = 8
KC = 16
SEQ = 2048
DEPTH = 4
ALPHA = (2 * DEPTH) ** 0.25
EPS = 1e-5
NEG = -1e30
D_FF = 5632
D_FFE = 2816
NE = 8
MEM = 256

ENG_ATTR = {"pe": "tensor", "act": "scalar", "dve": "vector", "pool": "gpsimd", "sp": "sync"}
_uid = [0]


_POOLS = {}


class SemPool:
    NDS = 4

    def __init__(self, nc, es):
        self.sems = []

        def newsem(nm):
            self.sems.append(es.enter_context(nc.semaphore(nm)))
            return len(self.sems) - 1
        self.csid = {e: newsem(f"c_{e}") for e in ("pe", "act", "dve", "pool")}
        self.ccount = {e: 0 for e in self.csid}
        self.dsid = {q: [newsem(f"d_{q}{k}") for k in range(self.NDS)] for q in ("sp", "act", "pool")}
        self.dcount = {q: 0 for q in self.dsid}
        self.known = {e: {} for e in ENG_ATTR}
        _POOLS[id(nc)] = self


class Stage:
    NDS = 4

    def __init__(self, nc, name):
        _uid[0] += 1
        self.nc, self.name = nc, f"{name}{_uid[0]}"
        self.es = ExitStack()
        self.streams = {e: [] for e in ENG_ATTR}
        pool = _POOLS[id(nc)]
        self.sems = pool.sems
        self.csid = pool.csid
        self.ccount = pool.ccount
        self.dsid = pool.dsid
        self.dcount = pool.dcount
        self.known = pool.known
        self.lastw = {}
        self.readers = {}
        self.pend = {e: ([], []) for e in ENG_ATTR}
        self.sbytes = 0

    def sb(self, nm, shape, dt):
        n = 1
        for s in shape[1:]:
            n *= s
        self.sbytes += n * (4 if dt == F32 else 2)
        return self.es.enter_context(self.nc.sbuf_tensor(f"{self.name}_{nm}", list(shape), dt))

    def ps(self, nm, shape, dt):
        return self.es.enter_context(self.nc.psum_tensor(f"{self.name}_{nm}", list(shape), dt))

    def _need(self, reads, writes):
        ev = {}

        def upd(e):
            if e is not None and ev.get(e[0], 0) < e[1]:
                ev[e[0]] = e[1]
        for r in reads:
            upd(self.lastw.get(r))
        for w in writes:
            upd(self.lastw.get(w))
            for sid, val in self.readers.get(w, {}).items():
                upd((sid, val))
        return ev

    def _wait(self, eng, ev):
        for sid, val in ev.items():
            if eng == "pe" and sid == self.csid["pe"]:
                continue
            if self.known[eng].get(sid, 0) >= val:
                continue
            self.known[eng][sid] = val
            sem = self.sems[sid]
            self.streams[eng].append(lambda e, sem=sem, val=val: e.wait_ge(sem, val))

    def _commit(self, ev, reads, writes):
        sid, val = ev
        for r in reads:
            d = self.readers.setdefault(r, {})
            if d.get(sid, 0) < val:
                d[sid] = val
        for w in writes:
            self.lastw[w] = ev
            self.readers[w] = {}

    def add(self, eng, fn, r=(), w=(), sig=True):
        self._wait(eng, self._need(r, w))
        pr, pw = self.pend[eng]
        if not sig:
            self.streams[eng].append(fn)
            pr.extend(r)
            pw.extend(w)
            return
        self.ccount[eng] += 1
        sid = self.csid[eng]
        sem = self.sems[sid]
        self.streams[eng].append(lambda e, fn=fn, sem=sem: fn(e).then_inc(sem, 1))
        self._commit((sid, self.ccount[eng]), list(r) + pr, list(w) + pw)
        self.pend[eng] = ([], [])

    def dma(self, q, out, in_, r=(), w=()):
        i = self.dcount[q]
        self.dcount[q] += 1
        sid = self.dsid[q][i % self.NDS]
        sem = self.sems[sid]
        ev = self._need(r, w)
        prev = 16 * (i // self.NDS)
        if prev > 0 and ev.get(sid, 0) < prev:
            ev[sid] = prev
        self._wait(q, ev)
        self.streams[q].append(lambda e, out=out, in_=in_, sem=sem: e.dma_start(out=out, in_=in_).then_inc(sem, 16))
        self._commit((sid, prev + 16), r, w)

    def wait_dmas(self, eng):
        ev = {}
        for q, sids in self.dsid.items():
            n = self.dcount[q]
            for k, sid in enumerate(sids):
                cnt = (n - k + self.NDS - 1) // self.NDS if n > k else 0
                if cnt > 0:
                    ev[sid] = 16 * cnt
        self._wait(eng, ev)

    def close(self):
        assert self.sbytes <= 150 * 1024, (self.name, self.sbytes)
        with self.nc.Block() as block:
            for eng, attr in ENG_ATTR.items():
                s = self.streams[eng]
                if not s:
                    continue

                def run(e, s=s):
                    for f in s:
                        f(e)
                getattr(block, attr)(run)
        self.es.close()


def MM(out, lhsT, rhs, start, stop):
    return lambda e: e.matmul(out, lhsT=lhsT, rhs=rhs, start=start, stop=stop)


def TR(out, in_, ident):
    return lambda e: e.transpose(out=out, in_=in_, identity=ident)


def ACTF(out, in_, func, bias=None, scale=None, accum=None):
    kw = {}
    if bias is not None:
        kw["bias"] = bias
    if scale is not None:
        kw["scale"] = scale
    if accum is not None:
        kw["accum_out"] = accum
    return lambda e: e.activation(out=out, in_=in_, func=func, **kw)


def TT(out, a, b, op):
    return lambda e: e.tensor_tensor(out=out, in0=a, in1=b, op=op)


def TS(out, a, s1, op0, s2=None, op1=None):
    if op1 is None:
        return lambda e: e.tensor_scalar(out=out, in0=a, scalar1=s1, scalar2=None, op0=op0)
    return lambda e: e.tensor_scalar(out=out, in0=a, scalar1=s1, scalar2=s2, op0=op0, op1=op1)


def STT(out, in0, scalar, in1, op0, op1):
    return lambda e: e.scalar_tensor_tensor(out=out, in0=in0, scalar=scalar, in1=in1, op0=op0, op1=op1)


def CP(out, in_):
    return lambda e: e.tensor_copy(out=out, in_=in_)


def MSET(ap, v):
    return lambda e: e.memset(ap, v)


def RED(out, in_, op):
    return lambda e: e.tensor_reduce(out=out, in_=in_, axis=AX.X, op=op)


def wview(W, k0, kc, n0, nb):
    return W[k0 * 128:(k0 + kc) * 128, n0:n0 + nb].rearrange("(c p) n -> p c n", p=128)


class Ctx:
    pass


def load_w(st, slot, key, W, k0, kc, n0, nb):
    for c in range(0, kc, 4):
        cc = min(4, kc - c)
        st.dma("pool", slot[:, c:c + cc, :nb], wview(W, k0 + c, cc, n0, nb), w=[(key, c)])


def wkeys(key, kc):
    return [(key, c) for c in range(0, kc, 4)]


def ln_tile(st, cx, Y, ykey, t, G, Bt, do_ln, h_out, tag):
    s = tag % 2
    if do_ln:
        st6 = cx.ln_st6[s]
        for c in range(4):
            st.add("dve", (lambda e, o=st6[:, c, :], i=Y[:, c * 512:(c + 1) * 512]: e.bn_stats(out=o, in_=i)),
                   r=[ykey], w=[("st6", s)])
        mv = cx.ln_mv[s]
        st.add("dve", (lambda e, o=mv[:, 0:2], i=st6[:]: e.bn_aggr(out=o, in_=i)), r=[("st6", s)], w=[("mv", s)])
        st.add("dve", TS(mv[:, 2:3], mv[:, 1:2], EPS, ALU.add), r=[("mv", s)], w=[("mv2", s)])
        st.add("pool", TT(mv[:, 2:3], mv[:, 2:3], cx.neghalf[:], ALU.pow), r=[("mv2", s)], w=[("mv2", s)])
        st.add("dve", STT(mv[:, 3:4], mv[:, 0:1], -1.0, mv[:, 2:3], ALU.mult, ALU.mult), r=[("mv", s), ("mv2", s)], w=[("mv3", s)])
        st.add("act", ACTF(Y, Y, AF.Identity, bias=mv[:, 3:4], scale=mv[:, 2:3]), r=[("mv2", s), ("mv3", s)], w=[ykey])
        st.add("dve", TT(Y, Y, G[:], ALU.mult), w=[ykey])
        st.add("pool", TT(Y, Y, Bt[:], ALU.add), w=[ykey])
    if h_out is not None:
        st.dma("sp", h_out[t * 128:(t + 1) * 128, :], Y, r=[ykey])
    HB = cx.ln_hb[s]
    st.add("act", ACTF(HB[:], Y, AF.Copy), r=[ykey], w=[("hb", s)])
    PTa, PTb = cx.ln_pt[s]
    for c in range(16):
        PT = PTa if c < 8 else PTb
        st.add("pe", TR(PT[:, c % 8, :], HB[:, c * 128:(c + 1) * 128], cx.ident[:]),
               r=[("hb", s)], w=[("pt", s, c // 8)], sig=(c % 8 == 7))
    st.add("dve", CP(cx.XT[:, 0:8, t * 128:(t + 1) * 128], PTa[:]), r=[("pt", s, 0)], w=[("xt", t)])
    st.add("act", ACTF(cx.XT[:, 8:16, t * 128:(t + 1) * 128], PTb[:], AF.Copy), r=[("pt", s, 1)], w=[("xt", t)])


def ln_alloc(st, cx, g_ap, b_ap, do_ln=True):
    cx.ln_st6 = [st.sb(f"st6{s}", [128, 4, 6], F32) for s in range(2)]
    cx.ln_mv = [st.sb(f"mv{s}", [128, 4], F32) for s in range(2)]
    cx.ln_hb = [st.sb(f"hb{s}", [128, D], BF16) for s in range(2)]
    cx.ln_pt = [(st.ps(f"pta{s}", [128, 8, 128], BF16), st.ps(f"ptb{s}", [128, 8, 128], BF16)) for s in range(2)]
    G = Bt = None
    if do_ln:
        G = st.sb("lng", [128, D], F32)
        Bt = st.sb("lnb", [128, D], F32)
        st.dma("sp", G[:], g_ap.partition_broadcast(128), w=["G"])
        st.dma("sp", Bt[:], b_ap.partition_broadcast(128), w=["B"])
    return G, Bt


def stage_load_x(cx, x_ap):
    st = Stage(cx.nc, "ldx")
    ln_alloc(st, cx, None, None, do_ln=False)
    Y = [st.sb(f"y{s}", [128, D], F32) for s in range(2)]
    for t in range(NT):
        s = t % 2
        st.dma("act", Y[s][:], x_ap[t * 128:(t + 1) * 128, :], w=[("y", s)])
        ln_tile(st, cx, Y[s][:], ("y", s), t, None, None, False, cx.hbuf, t)
    st.wait_dmas("sp")
    st.close()


def stage_proj_ln(cx, name, AT, kca, W, g_ap, b_ap, h_out=None, final=False):
    st = Stage(cx.nc, name)
    G, Bt = ln_alloc(st, cx, g_ap, b_ap)
    Yb = st.sb("Y", [128, 4, D], F32)
    WS = [st.sb(f"w{s}", [128, 16, 512], BF16) for s in range(2)]
    PS = [st.ps(f"ps{s}", [128, 512], F32) for s in range(4)]
    hout = cx.hbuf if h_out is None else h_out
    wi = 0
    pi = 0
    for tg in range(2):
        for tl in range(4):
            t = tg * 4 + tl
            st.dma("sp", Yb[:, tl, :], cx.hbuf[t * 128:(t + 1) * 128, :], w=[("Y", tl)])
        for nb in range(4):
            ws = wi % 2
            wi += 1
            assert kca <= 16
            load_w(st, WS[ws], ("w", ws), W, 0, kca, nb * 512, 512)
            for tl in range(4):
                t = tg * 4 + tl
                ps = PS[pi % 4]
                pk = ("ps", pi % 4)
                pi += 1
                for kc in range(kca):
                    st.add("pe", MM(ps[:], AT[:, kc, t * 128:(t + 1) * 128], WS[ws][:, kc, :], kc == 0, kc == kca - 1),
                           r=[("w", ws, kc // 4 * 4)] if False else [(("w", ws), kc // 4 * 4)], w=[pk], sig=(kc == kca - 1))
                ysl = Yb[:, tl, nb * 512:(nb + 1) * 512]
                st.add("dve", STT(ysl, ysl, ALPHA, ps[:], ALU.mult, ALU.add), r=[pk], w=[("Y", tl)])
        for tl in range(4):
            t = tg * 4 + tl
            ln_tile(st, cx, Yb[:, tl, :], ("Y", tl), t, G, Bt, True, hout, t)
    st.wait_dmas("sp")
    st.close()


def stage_mix_a(cx, AT, w_in, lng, lnb, w_s, b_s):
    st = Stage(cx.nc, "mixa")
    nc = cx.nc
    WS = [st.sb(f"w{s}", [128, 16, 512], BF16) for s in range(3)]
    WSp = st.sb("wsp", [128, 8, 128], BF16)
    WST = st.sb("wst", [128, 8, 128], BF16)
    bsB = st.sb("bsb", [128, 8, 128], F32)
    G1 = st.sb("g1", [128, 1024], F32)
    B1 = st.sb("b1", [128, 1024], F32)
    V32 = [st.sb(f"v32{s}", [128, 1024], F32) for s in range(2)]
    VLN = [st.sb(f"vln{s}", [128, 1024], BF16) for s in range(2)]
    SV = [st.sb(f"sv{s}", [128, 8, 128], F32) for s in range(2)]
    st6 = [st.sb(f"st6{s}", [128, 2, 6], F32) for s in range(2)]
    mv = [st.sb(f"mv{s}", [128, 4], F32) for s in range(2)]
    PS = [st.ps(f"ps{s}", [128, 512], F32) for s in range(4)]
    PG = [st.ps(f"pg{s}", [128, 8, 128], F32) for s in range(1)]
    PTw = st.ps("ptw", [128, 8, 128], BF16)

    st.dma("pool", WSp[:], w_s.rearrange("h p q -> p h q"), w=["wsp"])
    st.dma("sp", bsB[:].rearrange("p h q -> p (h q)"), b_s.rearrange("h q -> (h q)").partition_broadcast(128), w=["bsb"])
    st.dma("sp", G1[:], lng.partition_broadcast(128), w=["g1"])
    st.dma("sp", B1[:], lnb.partition_broadcast(128), w=["b1"])
    st.add("dve", MSET(WSp[0:64, :, 64:128], 0.0), w=["wsp"])
    for h in range(8):
        st.add("pe", TR(PTw[:, h, :], WSp[:, h, :], cx.ident[:]), r=["wsp"], w=["ptw"], sig=(h == 7))
    st.add("dve", CP(WST[:], PTw[:]), r=["ptw"], w=["wst"])

    wi = 0
    pi = 0
    for nb in range(2):
        ws = wi % 3
        wi += 1
        load_w(st, WS[ws], ("w", ws), w_in, 0, 16, nb * 512, 512)
        for oc in range(4):
            for th in range(2):
                ps = PS[pi % 4]
                pk = ("ps", pi % 4)
                pi += 1
                for kc in range(16):
                    st.add("pe", MM(ps[:], WS[ws][:, kc, oc * 128:(oc + 1) * 128], cx.XT[:, kc, th * 512:(th + 1) * 512], kc == 0, kc == 15),
                           r=[(("w", ws), kc // 4 * 4)], w=[pk], sig=(kc == 15))
                st.add("act", ACTF(AT[:, nb * 4 + oc, th * 512:(th + 1) * 512], ps[:], AF.Gelu), r=[pk], w=[("at", nb * 4 + oc)])
    wv = []
    for nb in range(2):
        ws = wi % 3
        wi += 1
        load_w(st, WS[ws], ("w", ws), w_in, 0, 16, 1024 + nb * 512, 512)
        wv.append(ws)
    for t in range(NT):
        s = t % 2
        for nb in range(2):
            ws = wv[nb]
            ps = PS[pi % 4]
            pk = ("ps", pi % 4)
            pi += 1
            for kc in range(16):
                st.add("pe", MM(ps[:], cx.XT[:, kc, t * 128:(t + 1) * 128], WS[ws][:, kc, :], kc == 0, kc == 15),
                       r=[(("w", ws), kc // 4 * 4)], w=[pk], sig=(kc == 15))
            st.add("act", ACTF(V32[s][:, nb * 512:(nb + 1) * 512], ps[:], AF.Gelu), r=[pk], w=[("v32", s)])
        for c in range(2):
            st.add("dve", (lambda e, o=st6[s][:, c, :], i=V32[s][:, c * 512:(c + 1) * 512]: e.bn_stats(out=o, in_=i)),
                   r=[("v32", s)], w=[("st6", s)])
        st.add("dve", (lambda e, o=mv[s][:, 0:2], i=st6[s][:]: e.bn_aggr(out=o, in_=i)), r=[("st6", s)], w=[("mv", s)])
        st.add("dve", TS(mv[s][:, 2:3], mv[s][:, 1:2], EPS, ALU.add), r=[("mv", s)], w=[("mv2", s)])
        st.add("pool", TT(mv[s][:, 2:3], mv[s][:, 2:3], cx.neghalf[:], ALU.pow), r=[("mv2", s)], w=[("mv2", s)])
        st.add("dve", STT(mv[s][:, 3:4], mv[s][:, 0:1], -1.0, mv[s][:, 2:3], ALU.mult, ALU.mult), r=[("mv", s), ("mv2", s)], w=[("mv3", s)])
        st.add("act", ACTF(V32[s][:], V32[s][:], AF.Identity, bias=mv[s][:, 3:4], scale=mv[s][:, 2:3]), r=[("mv2", s), ("mv3", s)], w=[("v32", s)])
        st.add("dve", TT(V32[s][:], V32[s][:], G1[:], ALU.mult), r=["g1"], w=[("v32", s)])
        st.add("pool", TT(VLN[s][:], V32[s][:], B1[:], ALU.add), r=["b1", ("v32", s)], w=[("vln", s)])
        pg = PG[0]
        for h in range(8):
            st.add("pe", MM(pg[:, h, :], VLN[s][:, h * 128:(h + 1) * 128], WST[:, h, :], True, True),
                   r=[("vln", s), "wst"], w=["pg"], sig=(h == 7))
        st.add("dve", TT(SV[s][:], pg[:], bsB[:], ALU.add), r=["pg", "bsb"], w=[("sv", s)])
        at_sl = AT[:, 0:8, t * 128:(t + 1) * 128]
        st.add("pool", TT(at_sl, SV[s][:], at_sl, ALU.mult), r=[("sv", s)] + [("at", c) for c in range(8)], w=[("ata", t)])
    st.close()


def dma_slow(st, q, out, in_, r=(), w=()):
    i = st.dcount[q]
    st.dcount[q] += 1
    sid = st.dsid[q][i % st.NDS]
    sem = st.sems[sid]
    ev = st._need(r, w)
    prev = 16 * (i // st.NDS)
    if prev > 0 and ev.get(sid, 0) < prev:
        ev[sid] = prev
    st._wait(q, ev)
    st.streams[q].append(lambda e, out=out, in_=in_, sem=sem: e.dma_start(out=out, in_=in_, allow_slow_non_contiguous=True).then_inc(sem, 16))
    st._commit((sid, prev + 16), r, w)


def stage_mix_b(cx, AT, w_in, w_pool, pool_scale, xh_ap):
    st = Stage(cx.nc, "mixb")
    N = 16 + T
    WS = [st.sb(f"w{s}", [128, 16, 256], BF16) for s in range(2)]
    WP = [st.sb(f"wp{s}", [128, 2, 256], BF16) for s in range(2)]
    XH = st.sb("xh", [16, D], F32)
    XHb = st.sb("xhb", [16, D], BF16)
    XHT = st.sb("xht", [128, 256], BF16)
    PSC = st.sb("psc", [128, 8], F32)
    A = st.sb("pa", [128, 2, N], F32)
    S = [st.sb(f"psum{s}", [128, 2, N], F32) for s in range(2)]
    TMP = st.sb("tmp", [128, 2, T], F32)
    PD = st.sb("pd", [128, 2, T], BF16)
    PS = [st.ps(f"ps{s}", [128, 512], F32) for s in range(4)]
    PH = st.ps("ph", [128, 512], F32)
    PTx = st.ps("ptx", [128, 1024], BF16)

    st.dma("sp", XH[:], xh_ap, w=["xh"])
    dma_slow(st, "sp", PSC[:], pool_scale.rearrange("(j p) -> p j", p=128), w=["psc"])
    st.add("act", ACTF(XHb[:], XH[:], AF.Copy), r=["xh"], w=["xhb"])
    for c in range(16):
        st.add("pe", TR(PTx[:, c * 16:(c + 1) * 16], XHb[:, c * 128:(c + 1) * 128], cx.ident[0:16, 0:16]), r=["xhb"], w=["ptx"], sig=(c == 15))
    st.add("dve", CP(XHT[:], PTx[:, 0:256]), r=["ptx"], w=["xht"])
    pi = 0
    for g in range(4):
        s = g % 2
        wwin = 2 ** (g + 1)
        load_w(st, WS[s], ("w", s), w_in, 0, 16, 2048 + g * 256, 256)
        st.dma("pool", WP[s][:], w_pool[g].rearrange("(c p) d -> p c d", p=128), w=[("wp", s)])
        for oc in range(2):
            for kc in range(16):
                st.add("pe", MM(PH[:, oc * 16:(oc + 1) * 16], WS[s][:, kc, oc * 128:(oc + 1) * 128], XHT[:, kc * 16:(kc + 1) * 16], kc == 0, kc == 15),
                       r=[(("w", s), kc // 4 * 4), "xht"], w=["ph"], sig=(kc == 15 and oc == 1))
        for oc in range(2):
            st.add("dve", TS(A[:, oc, 0:16], PH[:, oc * 16:(oc + 1) * 16], cx.pflag[:, 1:2], ALU.mult), r=["ph"], w=["pa"])
        for oc in range(2):
            for th in range(2):
                ps = PS[pi % 4]
                pk = ("ps", pi % 4)
                pi += 1
                for kc in range(16):
                    st.add("pe", MM(ps[:], WS[s][:, kc, oc * 128:(oc + 1) * 128], cx.XT[:, kc, th * 512:(th + 1) * 512], kc == 0, kc == 15),
                           r=[(("w", s), kc // 4 * 4)], w=[pk], sig=(kc == 15))
                st.add("act", ACTF(A[:, oc, 16 + th * 512:16 + (th + 1) * 512], ps[:], AF.Copy), r=[pk], w=["pa"])
        cur, ckey = A, "pa"
        sh = 1
        k = 0
        while sh < wwin:
            nxt, nkey = S[k % 2], ("s", k % 2)
            st.add("dve", TT(nxt[:, :, sh:N], cur[:, :, sh:N], cur[:, :, 0:N - sh], ALU.add), r=[ckey], w=[nkey])
            cur, ckey = nxt, nkey
            sh *= 2
            k += 1
        st.add("dve", TS(TMP[:], cur[:, :, 16:N], 1.0 / wwin, ALU.mult), r=[ckey], w=["tmp"])
        for oc in range(2):
            st.add("dve", TT(TMP[:, oc, 0:16], cur[:, oc, 16:32], cx.pcorr[:, g, :], ALU.mult), r=[ckey], w=["tmp"])
        st.add("pool", TT(PD[:], TMP[:], A[:, :, 16:N], ALU.subtract), r=["tmp", "pa"], w=["pd"])
        for dc in range(2):
            for th in range(2):
                ps = PS[pi % 4]
                pk = ("ps", pi % 4)
                pi += 1
                for c in range(2):
                    st.add("pe", MM(ps[:], WP[s][:, c, dc * 128:(dc + 1) * 128], PD[:, c, th * 512:(th + 1) * 512], c == 0, c == 1),
                           r=[("wp", s), "pd"], w=[pk], sig=(c == 1))
                j = g * 2 + dc
                st.add("act", ACTF(AT[:, 8 + j, th * 512:(th + 1) * 512], ps[:], AF.Copy, scale=PSC[:, j:j + 1]), r=[pk, "psc"], w=[("at", 8 + j)])
    st.close()


def stage_xkv(cx, KTm, VM, mem_ap, w_kv):
    st = Stage(cx.nc, "xkv")
    M32 = st.sb("m32", [128, 2, D], F32)
    Mb = st.sb("mb", [128, 2, D], BF16)
    MT = st.sb("mt", [128, 16, 256], BF16)
    WS = [st.sb(f"w{s}", [128, 16, 512], BF16) for s in range(2)]
    PS = [st.ps(f"ps{s}", [128, 512], F32) for s in range(4)]
    PT = [st.ps(f"pt{s}", [128, 8, 128], BF16) for s in range(2)]
    for j in range(2):
        st.dma("sp", M32[:, j, :], mem_ap[j * 128:(j + 1) * 128, :], w=[("m32", j)])
        st.add("act", ACTF(Mb[:, j, :], M32[:, j, :], AF.Copy), r=[("m32", j)], w=[("mb", j)])
        for c in range(16):
            st.add("pe", TR(PT[c // 8][:, c % 8, :], Mb[:, j, c * 128:(c + 1) * 128], cx.ident[:]),
                   r=[("mb", j)], w=[("pt", c // 8)], sig=(c % 8 == 7))
        st.add("dve", CP(MT[:, 0:8, j * 128:(j + 1) * 128], PT[0][:]), r=[("pt", 0)], w=[("mt", j)])
        st.add("act", ACTF(MT[:, 8:16, j * 128:(j + 1) * 128], PT[1][:], AF.Copy), r=[("pt", 1)], w=[("mt", j)])
    wi = 0
    pi = 0
    for nb in range(4):
        ws = wi % 2
        wi += 1
        load_w(st, WS[ws], ("w", ws), w_kv, 0, 16, nb * 512, 512)
        for oc in range(4):
            ps = PS[pi % 4]
            pk = ("ps", pi % 4)
            pi += 1
            for kc in range(16):
                st.add("pe", MM(ps[:, 0:256], WS[ws][:, kc, oc * 128:(oc + 1) * 128], MT[:, kc, :], kc == 0, kc == 15),
                       r=[(("w", ws), kc // 4 * 4), ("mt", 0), ("mt", 1)], w=[pk], sig=(kc == 15))
            st.add("act", ACTF(KTm[:, nb * 4 + oc, :], ps[:, 0:256], AF.Copy), r=[pk], w=[("ktm", nb * 4 + oc)])
    for nb in range(4):
        ws = wi % 2
        wi += 1
        load_w(st, WS[ws], ("w", ws), w_kv, 0, 16, D + nb * 512, 512)
        for j in range(2):
            ps = PS[pi % 4]
            pk = ("ps", pi % 4)
            pi += 1
            for kc in range(16):
                st.add("pe", MM(ps[:], MT[:, kc, j * 128:(j + 1) * 128], WS[ws][:, kc, :], kc == 0, kc == 15),
                       r=[(("w", ws), kc // 4 * 4), ("mt", 0), ("mt", 1)], w=[pk], sig=(kc == 15))
            st.add("dve", CP(VM[:, j, nb * 512:(nb + 1) * 512], ps[:]), r=[pk], w=[("vm", j, nb)])
    st.close()


def stage_fm_proj(cx, name, OUT, W, n0, nblocks, scale):
    st = Stage(cx.nc, name)
    WS = [st.sb(f"w{s}", [128, 16, 512], BF16) for s in range(2)]
    PS = [st.ps(f"ps{s}", [128, 512], F32) for s in range(4)]
    pi = 0
    for nb in range(nblocks):
        ws = nb % 2
        load_w(st, WS[ws], ("w", ws), W, 0, 16, n0 + nb * 512, 512)
        for oc in range(4):
            for th in range(2):
                ps = PS[pi % 4]
                pk = ("ps", pi % 4)
                pi += 1
                for kc in range(16):
                    st.add("pe", MM(ps[:], WS[ws][:, kc, oc * 128:(oc + 1) * 128], cx.XT[:, kc, th * 512:(th + 1) * 512], kc == 0, kc == 15),
                           r=[(("w", ws), kc // 4 * 4)], w=[pk], sig=(kc == 15))
                osl = OUT[:, nb * 4 + oc, th * 512:(th + 1) * 512]
                if pi % 2 == 0:
                    st.add("act", ACTF(osl, ps[:], AF.Copy, scale=scale), r=[pk], w=[("o", nb * 4 + oc, th)])
                else:
                    st.add("dve", TS(osl, ps[:], scale, ALU.mult), r=[pk], w=[("o", nb * 4 + oc, th)])
    st.close()


def stage_xatt(cx, QT, KTm, VM, AT):
    st = Stage(cx.nc, "xatt")
    PE_ = st.sb("pexp", [128, 4, 256], F32)
    PN = st.sb("pn", [128, 4, 256], BF16)
    PTs = [st.sb(f"pts{s}", [128, 1024], BF16) for s in range(2)]
    MX = st.sb("mx", [128, 8], F32)
    SM = st.sb("sm", [128, 8], F32)
    PSS = st.ps("pss", [128, 4, 256], F32)
    PTp = st.ps("ptp", [128, 1024], BF16)
    PSO = [st.ps(f"pso{s}", [128, 512], F32) for s in range(4)]
    it = 0
    po = 0
    for tg in range(2):
        for h in range(4):
            s = it % 2
            it += 1
            for tl in range(4):
                t = tg * 4 + tl
                for dc in range(4):
                    st.add("pe", MM(PSS[:, tl, :], QT[:, h * 4 + dc, t * 128:(t + 1) * 128], KTm[:, h * 4 + dc, :], dc == 0, dc == 3),
                           w=["pss"], sig=(dc == 3 and tl == 3))
            st.add("dve", RED(MX[:, 0:4], PSS[:], ALU.max), r=["pss"], w=["mx"])
            st.add("dve", TS(MX[:, 4:8], MX[:, 0:4], -1.0, ALU.mult), r=["mx"], w=["nmx"])
            st.add("dve", MSET(SM[:], 0.0), w=["sm"])
            for tl in range(4):
                st.add("act", ACTF(PE_[:, tl, :], PSS[:, tl, :], AF.Exp, bias=MX[:, 4 + tl:5 + tl], accum=SM[:, tl:tl + 1]),
                       r=["pss", "nmx"], w=["pexp", "sm"])
            st.add("dve", (lambda e, o=SM[:, 4:8], i=SM[:, 0:4]: e.reciprocal(out=o, in_=i)), r=["sm"], w=["rs"])
            for tl in range(4):
                st.add("dve", TS(PN[:, tl, :], PE_[:, tl, :], SM[:, 4 + tl:5 + tl], ALU.mult), r=["pexp", "rs"], w=["pn"])
            for tl in range(4):
                for mk in range(2):
                    st.add("pe", TR(PTp[:, (mk * 4 + tl) * 128:(mk * 4 + tl + 1) * 128], PN[:, tl, mk * 128:(mk + 1) * 128], cx.ident[:]),
                           r=["pn"], w=["ptp"], sig=(tl == 3 and mk == 1))
            st.add("act", ACTF(PTs[s][:], PTp[:], AF.Copy), r=["ptp"], w=[("pts", s)])
            for dc in range(4):
                ps = PSO[po % 4]
                pk = ("pso", po % 4)
                po += 1
                for mk in range(2):
                    st.add("pe", MM(ps[:], VM[:, mk, h * 512 + dc * 128:h * 512 + (dc + 1) * 128], PTs[s][:, mk * 512:(mk + 1) * 512], mk == 0, mk == 1),
                           r=[("pts", s)], w=[pk], sig=(mk == 1))
                osl = AT[:, h * 4 + dc, tg * 512:(tg + 1) * 512]
                if dc % 2 == 0:
                    st.add("dve", CP(osl, ps[:]), r=[pk], w=[("at", h * 4 + dc, tg)])
                else:
                    st.add("act", ACTF(osl, ps[:], AF.Copy), r=[pk], w=[("at", h * 4 + dc, tg)])
    st.close()


def stage_dqkv(cx, w_qkv, qT_d, kT_d, v_d):
    st = Stage(cx.nc, "dqkv")
    WS = [st.sb(f"w{s}", [128, 16, 512], BF16) for s in range(2)]
    SG = [st.sb(f"sg{s}", [128, 4, T], BF16) for s in range(2)]
    VS = [st.sb(f"vs{s}", [128, 8, 512], BF16) for s in range(2)]
    PS = [st.ps(f"ps{s}", [128, 512], F32) for s in range(4)]
    pi = 0
    wi = 0
    for nb in range(8):
        ws = wi % 2
        wi += 1
        s = nb % 2
        load_w(st, WS[ws], ("w", ws), w_qkv, 0, 16, nb * 512, 512)
        sc = 0.125 if nb < 4 else 1.0
        for oc in range(4):
            for th in range(2):
                ps = PS[pi % 4]
                pk = ("ps", pi % 4)
                pi += 1
                for kc in range(16):
                    st.add("pe", MM(ps[:], WS[ws][:, kc, oc * 128:(oc + 1) * 128], cx.XT[:, kc, th * 512:(th + 1) * 512], kc == 0, kc == 15),
                           r=[(("w", ws), kc // 4 * 4)], w=[pk], sig=(kc == 15))
                osl = SG[s][:, oc, th * 512:(th + 1) * 512]
                if pi % 2 == 0:
                    st.add("act", ACTF(osl, ps[:], AF.Copy, scale=sc), r=[pk], w=[("sg", s)])
                else:
                    st.add("dve", TS(osl, ps[:], sc, ALU.mult), r=[pk], w=[("sg", s)])
        dst = qT_d if nb < 4 else kT_d
        r0 = (nb % 4) * 512
        st.dma("sp", dst[r0:r0 + 512, :].rearrange("(c p) t -> p c t", p=128), SG[s][:], r=[("sg", s)])
    for nb in range(4):
        ws = wi % 2
        wi += 1
        s = nb % 2
        load_w(st, WS[ws], ("w", ws), w_qkv, 0, 16, 2 * D + nb * 512, 512)
        for t in range(NT):
            ps = PS[pi % 4]
            pk = ("ps", pi % 4)
            pi += 1
            for kc in range(16):
                st.add("pe", MM(ps[:], cx.XT[:, kc, t * 128:(t + 1) * 128], WS[ws][:, kc, :], kc == 0, kc == 15),
                       r=[(("w", ws), kc // 4 * 4)], w=[pk], sig=(kc == 15))
            if t % 2 == 0:
                st.add("act", ACTF(VS[s][:, t, :], ps[:], AF.Copy), r=[pk], w=[("vs", s)])
            else:
                st.add("dve", CP(VS[s][:, t, :], ps[:]), r=[pk], w=[("vs", s)])
        st.dma("sp", v_d[:, nb * 512:(nb + 1) * 512].rearrange("(t p) n -> p t n", p=128), VS[s][:], r=[("vs", s)])
    st.wait_dmas("sp")
    st.close()


def stage_datt(cx, AT, qT_d, kT_d, v_d, kTp_d, vp_d, rel_table, lq1, lk1, lq2, lk2, subg, lam_init):
    st = Stage(cx.nc, "datt")
    RT = st.sb("rt", [128, 512], F32)
    BH = st.sb("bh", [128, 16, 256], F32)
    MSK = [st.sb(f"msk{s}", [128, 256], F32) for s in range(2)]
    LV = st.sb("lv", [128, 4, 64], F32)
    LS = st.sb("ls", [128, 8], F32)
    GS = st.sb("gs", [128, 128], F32)
    QH = [st.sb(f"qh{s}", [128, T], BF16) for s in range(2)]
    KH = [st.sb(f"kh{s}", [128, 2 * T], BF16) for s in range(2)]
    VH = [st.sb(f"vh{s}", [128, 16, 128], BF16) for s in range(2)]
    P = [st.sb(f"p{m}", [128, 2 * T], BF16) for m in range(2)]
    TN = st.sb("tn", [128, 256], F32)
    TMP = st.sb("tmp", [128, 2 * T], F32)
    PD = st.sb("pd", [128, 2 * T], BF16)
    PDT = st.sb("pdt", [128, 2 * T], BF16)
    SC = st.sb("sc", [128, 16], F32)
    SM = st.sb("sm", [128, 2, 4], F32)
    ON = st.sb("on", [128, 128], BF16)
    SQ = st.sb("sq", [128, 128], F32)
    S = st.ps("s", [128, 2 * T], F32)
    PTp = st.ps("ptp", [128, 2 * T], BF16)
    O = st.ps("o", [128, 512], F32)
    PTo = st.ps("pto", [128, 1024], BF16)

    st.dma("sp", RT[:], rel_table.rearrange("b h -> (b h)").partition_broadcast(128), w=["rt"])
    for k, lv in enumerate((lq1, lk1, lq2, lk2)):
        st.dma("sp", LV[:, k, :], lv.partition_broadcast(128), w=[("lv", k)])
    st.dma("sp", GS[:], subg.partition_broadcast(128), w=["gs"])
    st.add("dve", TT(LV[:, 0, :], LV[:, 0, :], LV[:, 1, :], ALU.mult), r=[("lv", 1)], w=[("lv", 0)])
    st.add("dve", TT(LV[:, 2, :], LV[:, 2, :], LV[:, 3, :], ALU.mult), r=[("lv", 3)], w=[("lv", 2)])
    st.add("dve", RED(LS[:, 0:1], LV[:, 0, :], ALU.add), r=[("lv", 0)], w=["ls0"])
    st.add("dve", RED(LS[:, 1:2], LV[:, 2, :], ALU.add), r=[("lv", 2)], w=["ls1"])
    st.add("act", ACTF(LS[:, 2:4], LS[:, 0:2], AF.Exp), r=["ls0", "ls1"], w=["ls2"])
    st.add("dve", TT(LS[:, 4:5], LS[:, 3:4], LS[:, 2:3], ALU.subtract), r=["ls2"], w=["ls4"])
    st.add("dve", TS(LS[:, 5:6], LS[:, 4:5], -lam_init, ALU.add), r=["ls4"], w=["nlam"])
    NLAM = LS[:, 5:6]
    st.add("act", ACTF(GS[:], GS[:], AF.Copy, scale=(1.0 - lam_init)), w=["gs"])
    for h in range(16):
        st.add("pool", CP(BH[:, h, :], cx.nmask[:]), w=[("bh", h)])
    for b in range(32):
        s = b % 2
        st.add("dve", TS(MSK[s][:], cx.relidx[:], float(b), ALU.is_equal), w=[("msk", s)])
        for h in range(16):
            eng = "dve"
            st.add(eng, STT(BH[:, h, :], MSK[s][:], RT[:, b * 16 + h:b * 16 + h + 1], BH[:, h, :], ALU.mult, ALU.add),
                   r=[("msk", s), "rt"], w=[("bh", h)])
    CH = RT[:, 15 * 16:16 * 16]
    PF = cx.pflag[:, 0:1]

    for h in range(16):
        s = h % 2
        rows = slice(h * 128, (h + 1) * 128)
        st.dma("sp", QH[s][:], qT_d[rows, :], w=[("qh", s)])
        st.dma("sp", KH[s][:, 0:T], kTp_d[rows, :], w=[("kh", s, 0)])
        st.dma("sp", KH[s][:, T:2 * T], kT_d[rows, :], w=[("kh", s, 1)])
        st.dma("act", VH[s][:, 0:8, :], vp_d[:, rows].rearrange("(t p) e -> p t e", p=128), w=[("vh", s, 0)])
        st.dma("act", VH[s][:, 8:16, :], v_d[:, rows].rearrange("(t p) e -> p t e", p=128), w=[("vh", s, 1)])
        for j in range(NT):
            nk = T + 128 * (j + 1)
            nf = nk - 256
            st.add("dve", MSET(SM[:], 0.0), w=["sm"])
            for m in range(2):
                nb = (nk + 511) // 512
                for kb in range(nb):
                    c0 = kb * 512
                    c1 = min(nk, c0 + 512)
                    st.add("pe", MM(S[:, c0:c1], QH[s][m * 64:(m + 1) * 64, j * 128:(j + 1) * 128], KH[s][m * 64:(m + 1) * 64, c0:c1], True, True),
                           r=[("qh", s), ("kh", s, 0), ("kh", s, 1)], w=["S"], sig=(kb == nb - 1))
                st.add("dve", RED(SC[:, m:m + 1], S[:, 0:nk], ALU.max), r=["S"], w=[("mx", m)])
                st.add("dve", TS(SC[:, 2 + m:3 + m], SC[:, m:m + 1], -1.0, ALU.mult), r=[("mx", m)], w=[("nmx", m)])
                st.add("dve", TT(SC[:, 4 + m:5 + m], SC[:, 2 + m:3 + m], CH[:, h:h + 1], ALU.add), r=[("nmx", m), "rt"], w=[("fb", m)])
                st.add("dve", TT(SC[:, 6 + m:7 + m], SC[:, 4 + m:5 + m], PF, ALU.add), r=[("fb", m)], w=[("fbp", m)])
                pf_end = min(nf, T)
                st.add("act", ACTF(P[m][:, 0:pf_end], S[:, 0:pf_end], AF.Exp, bias=SC[:, 6 + m:7 + m], accum=SM[:, m, 0:1]),
                       r=["S", ("fbp", m)], w=[("p", m), "sm"])
                if nf > T:
                    st.add("act", ACTF(P[m][:, T:nf], S[:, T:nf], AF.Exp, bias=SC[:, 4 + m:5 + m], accum=SM[:, m, 1:2]),
                           r=["S", ("fb", m)], w=[("p", m), "sm"])
                st.add("dve", TT(TN[:], S[:, nf:nk], BH[:, h, :], ALU.add), r=["S", ("bh", h)], w=["tn"])
                if j == 0:
                    st.add("dve", TS(TN[:, 0:128], TN[:, 0:128], PF, ALU.add), w=["tn"])
                st.add("act", ACTF(P[m][:, nf:nk], TN[:], AF.Exp, bias=SC[:, 2 + m:3 + m], accum=SM[:, m, 2:3]),
                       r=["tn", ("nmx", m)], w=[("p", m), "sm"])
            st.add("dve", RED(SC[:, 8:10], SM[:], ALU.add), r=["sm"], w=["ssum"])
            st.add("dve", (lambda e, o=SC[:, 10:12], i=SC[:, 8:10]: e.reciprocal(out=o, in_=i)), r=["ssum"], w=["rr"])
            st.add("dve", TT(SC[:, 12:13], SC[:, 11:12], NLAM, ALU.mult), r=["rr", "nlam"], w=["c1"])
            st.add("pool", TS(TMP[:, 0:nk], P[1][:, 0:nk], SC[:, 12:13], ALU.mult), r=[("p", 1), "c1"], w=["tmp"])
            st.add("dve", STT(PD[:, 0:nk], P[0][:, 0:nk], SC[:, 10:11], TMP[:, 0:nk], ALU.mult, ALU.add), r=[("p", 0), "rr", "tmp"], w=["pd"])
            nkt = nk // 128
            for kt in range(nkt):
                last = (kt % 8 == 7) or (kt == nkt - 1)
                st.add("pe", TR(PTp[:, kt * 128:(kt + 1) * 128], PD[:, kt * 128:(kt + 1) * 128], cx.ident[:]),
                       r=["pd"], w=[("ptp", kt // 8)], sig=last)
            st.add("act", ACTF(PDT[:, 0:1024], PTp[:, 0:1024], AF.Copy), r=[("ptp", 0)], w=[("pdt", 0)])
            st.add("dve", CP(PDT[:, 1024:nk], PTp[:, 1024:nk]), r=[("ptp", 1)], w=[("pdt", 1)])
            for kt in range(nkt):
                st.add("pe", MM(O[:, 0:128], PDT[:, kt * 128:(kt + 1) * 128], VH[s][:, kt, :], kt == 0, kt == nkt - 1),
                       r=[("pdt", 0), ("pdt", 1), ("vh", s, 0), ("vh", s, 1)], w=["O"], sig=(kt == nkt - 1))
            st.add("dve", MSET(SC[:, 13:14], 0.0), w=["ssq"])
            st.add("act", ACTF(SQ[:], O[:, 0:128], AF.Square, accum=SC[:, 13:14]), r=["O"], w=["sq", "ssq"])
            st.add("dve", TS(SC[:, 14:15], SC[:, 13:14], 1.0 / 128, ALU.mult, EPS, ALU.add), r=["ssq"], w=["rr2a"])
            st.add("pool", TT(SC[:, 15:16], SC[:, 14:15], cx.neghalf[:], ALU.pow), r=["rr2a"], w=["rr2"])
            st.add("dve", STT(ON[:], O[:, 0:128], SC[:, 15:16], GS[:], ALU.mult, ALU.mult), r=["O", "rr2", "gs"], w=["on"])
            st.add("pe", TR(PTo[:, 0:128], ON[:], cx.ident[:]), r=["on"], w=["pto"])
            st.add("act", ACTF(AT[:, h, j * 128:(j + 1) * 128], PTo[:, 0:128], AF.Copy), r=["pto"], w=[("at", h, j)])
    st.close()


def stage_acc_load(cx, ACC, GATE, wrT):
    st = Stage(cx.nc, "accld")
    for t in range(NT):
        st.dma("sp" if t % 2 == 0 else "act", ACC[:, t, :], cx.hbuf[t * 128:(t + 1) * 128, :], w=[("acc", t)])
    if wrT is not None:
        WR = [st.sb(f"wr{s}", [128, D], F32) for s in range(2)]
        JK = [st.sb(f"jk{k}", [128, D], F32) for k in range(2)]
        LG = st.sb("lg", [128, NT, 8], F32)
        M8 = st.sb("m8", [128, NT, 8], F32)
        NM = st.sb("nm", [128, NT], F32)
        EX = st.sb("ex", [128, NT, 8], F32)
        MK = st.sb("mk", [128, NT, 8], F32)
        DN = st.sb("dn", [128, NT, 2], F32)
        st.add("dve", MSET(LG[:], 0.0), w=["lg"])
        for e in range(NE):
            s = e % 2
            st.dma("sp", WR[s][:], wrT[e].partition_broadcast(128), w=[("wr", s)])
            for t in range(NT):
                k = t % 2
                st.add("pool", TT(JK[k][:], ACC[:, t, :], WR[s][:], ALU.mult), r=[("acc", t), ("wr", s)], w=[("jk", k)])
                st.add("dve", RED(LG[:, t, e:e + 1], JK[k][:], ALU.add), r=[("jk", k)], w=["lg"])
        for t in range(NT):
            st.add("dve", (lambda en, o=M8[:, t, :], i=LG[:, t, :]: en.max(out=o, in_=i)), r=["lg"], w=["m8"])
        st.add("dve", TS(NM[:], M8[:, :, 0], -1.0, ALU.mult), r=["m8"], w=["nm"])
        for t in range(NT):
            st.add("act", ACTF(EX[:, t, :], LG[:, t, :], AF.Exp, bias=NM[:, t:t + 1]), r=["lg", "nm"], w=["ex"])
            st.add("dve", TS(MK[:, t, :], LG[:, t, :], M8[:, t, 1:2], ALU.is_ge), r=["lg", "m8"], w=["mk"])
        st.add("dve", TT(EX[:], EX[:], MK[:], ALU.mult), r=["mk", "ex"], w=["ex"])
        st.add("dve", RED(DN[:, :, 0], EX[:], ALU.add), r=["ex"], w=["dn"])
        st.add("dve", (lambda en, o=DN[:, :, 1], i=DN[:, :, 0]: en.reciprocal(out=o, in_=i)), r=["dn"], w=["dn2"])
        for t in range(NT):
            st.add("dve", TS(GATE[:, t, :], EX[:, t, :], DN[:, t, 1:2], ALU.mult), r=["ex", "dn2"], w=["gate"])
    for t in range(NT):
        st.add("act", ACTF(ACC[:, t, :], ACC[:, t, :], AF.Copy, scale=ALPHA), r=["lg"] if wrT is not None else [], w=[("acc", t)])
    st.close()


def stage_ffn(cx, ACC, GATE, experts, nchunks):
    st = Stage(cx.nc, "ffn")
    WS = [st.sb(f"w{s}", [128, 16, 512], BF16) for s in range(3)]
    WD = st.sb("wd", [128, 4, D], BF16)
    HID = st.sb("hid", [128, 4, T], BF16)
    SGt = [st.sb(f"sg{s}", [128, 512], F32) for s in range(2)]
    PG = [st.ps(f"pg{s}", [128, 512], F32) for s in range(2)]
    PU = [st.ps(f"pu{s}", [128, 512], F32) for s in range(2)]
    PD_ = [st.ps(f"pd{s}", [128, 512], F32) for s in range(4)]
    wi = 0
    gi = 0
    di = 0
    blocks = []
    c = 0
    while c < nchunks:
        cn = min(4, nchunks - c)
        blocks.append((c, cn))
        c += cn
    for e, (Wg, Wu, Wd) in enumerate(experts):
        for (c0, cn) in blocks:
            wg = wi % 3
            wu = (wi + 1) % 3
            wi += 2
            load_w(st, WS[wg], ("w", wg), Wg, 0, 16, c0 * 128, cn * 128)
            load_w(st, WS[wu], ("w", wu), Wu, 0, 16, c0 * 128, cn * 128)
            st.dma("pool", WD[:, 0:cn, :], Wd[c0 * 128:(c0 + cn) * 128, :].rearrange("(c p) n -> p c n", p=128), w=["wd"])
            for oc in range(cn):
                for th in range(2):
                    s = gi % 2
                    gi += 1
                    for kc in range(16):
                        st.add("pe", MM(PG[s][:], WS[wg][:, kc, oc * 128:(oc + 1) * 128], cx.XT[:, kc, th * 512:(th + 1) * 512], kc == 0, kc == 15),
                               r=[(("w", wg), kc // 4 * 4)], w=[("pg", s)], sig=(kc == 15))
                    for kc in range(16):
                        st.add("pe", MM(PU[s][:], WS[wu][:, kc, oc * 128:(oc + 1) * 128], cx.XT[:, kc, th * 512:(th + 1) * 512], kc == 0, kc == 15),
                               r=[(("w", wu), kc // 4 * 4)], w=[("pu", s)], sig=(kc == 15))
                    st.add("act", ACTF(SGt[s][:], PG[s][:], AF.Silu), r=[("pg", s)], w=[("sg", s)])
                    st.add("dve", TT(HID[:, oc, th * 512:(th + 1) * 512], SGt[s][:], PU[s][:], ALU.mult), r=[("sg", s), ("pu", s)], w=[("hid", oc, th)])
            for t in range(NT):
                for nb in range(4):
                    ps = PD_[di % 4]
                    pk = ("pd", di % 4)
                    di += 1
                    for cc in range(cn):
                        st.add("pe", MM(ps[:], HID[:, cc, t * 128:(t + 1) * 128], WD[:, cc, nb * 512:(nb + 1) * 512], cc == 0, cc == cn - 1),
                               r=["wd"] + [("hid", cc, t // 4)], w=[pk], sig=(cc == cn - 1))
                    asl = ACC[:, t, nb * 512:(nb + 1) * 512]
                    if GATE is None:
                        st.add("dve", TT(asl, asl, ps[:], ALU.add), r=[pk], w=[("acc", t, nb)])
                    else:
                        st.add("dve", STT(asl, ps[:], GATE[:, t, e:e + 1], asl, ALU.mult, ALU.add), r=[pk], w=[("acc", t, nb)])
    st.close()


def stage_ln_acc(cx, ACC, g_ap, b_ap, h_out=None):
    st = Stage(cx.nc, "lnacc")
    G, Bt = ln_alloc(st, cx, g_ap, b_ap)
    hout = cx.hbuf if h_out is None else h_out
    for t in range(NT):
        ln_tile(st, cx, ACC[:, t, :], ("acc", t), t, G, Bt, True, hout, t)
    st.wait_dmas("sp")
    st.close()


class Prog:
    def __init__(self):
        self.nc = bass.Bass("TRN2", target_bir_lowering=False)
        self.ins = {}
        self.outs = {}

    def inp(self, name, shape, dt=F32):
        if name not in self.ins:
            self.ins[name] = self.nc.dram_tensor(name, list(shape), dt, kind="ExternalInput").ap()
        return self.ins[name]

    def out(self, name, shape, dt=F32):
        if name not in self.outs:
            self.outs[name] = self.nc.dram_tensor(name, list(shape), dt, kind="ExternalOutput").ap()
        return self.outs[name]


def lam_init_of(layer):
    return 0.8 - 0.6 * math.exp(-0.3 * layer)


def emit_xattn(cx, pg, l, mem_ap):
    nc = cx.nc
    w_q = pg.inp(f"xa_w_q_{l}", [D, D])
    w_kv = pg.inp(f"xa_w_kv_{l}", [D, 2 * D])
    w_o = pg.inp(f"xa_w_o_{l}", [D, D])
    ln_g = pg.inp("ln_g", [DEPTH, 3, D])
    ln_b = pg.inp("ln_b", [DEPTH, 3, D])
    with ExitStack() as les:
        AT = les.enter_context(nc.sbuf_tensor(f"xAT{l}", [128, 16, T], BF16))
        with ExitStack() as es2:
            QT = es2.enter_context(nc.sbuf_tensor(f"xQT{l}", [128, 16, T], BF16))
            KTm = es2.enter_context(nc.sbuf_tensor(f"xKT{l}", [128, 16, MEM], BF16))
            VM = es2.enter_context(nc.sbuf_tensor(f"xVM{l}", [128, 2, D], BF16))
            stage_xkv(cx, KTm, VM, mem_ap, w_kv)
            stage_fm_proj(cx, "xq", QT, w_q, 0, 4, 512 ** -0.5)
            stage_xatt(cx, QT, KTm, VM, AT)
        stage_proj_ln(cx, "xo", AT, 16, w_o, ln_g[l, 1], ln_b[l, 1])


def emit_ffn(cx, pg, l):
    nc = cx.nc
    i = l // 2
    ln_g = pg.inp("ln_g", [DEPTH, 3, D])
    ln_b = pg.inp("ln_b", [DEPTH, 3, D])
    with ExitStack() as les:
        ACC = les.enter_context(nc.sbuf_tensor(f"ACC{l}", [128, NT, D], F32))
        if l % 2 == 0:
            ex = [(pg.inp(f"ffn_w_gate_{i}", [D, D_FF]), pg.inp(f"ffn_w_up_{i}", [D, D_FF]), pg.inp(f"ffn_w_down_{i}", [D_FF, D]))]
            stage_acc_load(cx, ACC, None, None)
            stage_ffn(cx, ACC, None, ex, D_FF // 128)
        else:
            GATE = les.enter_context(nc.sbuf_tensor(f"GATE{l}", [128, NT, NE], F32))
            wrT = pg.inp(f"moe_w_routerT_{i}", [NE, D])
            ex = [(pg.inp(f"moe_w_gate_{i}_{e}", [D, D_FFE]), pg.inp(f"moe_w_up_{i}_{e}", [D, D_FFE]),
                   pg.inp(f"moe_w_down_{i}_{e}", [D_FFE, D])) for e in range(NE)]
            stage_acc_load(cx, ACC, GATE, wrT)
            stage_ffn(cx, ACC, GATE, ex, D_FFE // 128)
        stage_ln_acc(cx, ACC, ln_g[l, 2], ln_b[l, 2])


def emit_even(cx, pg, l, xh_ap, mem_ap):
    nc = cx.nc
    i = l // 2
    w_in = pg.inp(f"mix_w_in_{i}", [D, 3072])
    ln_g = pg.inp("ln_g", [DEPTH, 3, D])
    ln_b = pg.inp("ln_b", [DEPTH, 3, D])
    with ExitStack() as les:
        AT = les.enter_context(nc.sbuf_tensor(f"mAT{l}", [128, 16, T], BF16))
        stage_mix_a(cx, AT, w_in, pg.inp(f"gmlp_ln_g_{i}", [1024]), pg.inp(f"gmlp_ln_b_{i}", [1024]),
                    pg.inp(f"gmlp_w_s_{i}", [8, 128, 128]), pg.inp(f"gmlp_b_s_{i}", [8, 128]))
        stage_mix_b(cx, AT, w_in, pg.inp(f"pool_w_{i}", [4, 256, 256]), pg.inp(f"pool_scale_{i}", [1024]), xh_ap)
        stage_proj_ln(cx, "mixo", AT, 16, pg.inp(f"mix_w_out_{i}", [D, D]), ln_g[l, 0], ln_b[l, 0])
    emit_xattn(cx, pg, l, mem_ap)
    emit_ffn(cx, pg, l)


def emit_odd_qkv(cx, pg, l, qT_d, kT_d, v_d):
    i = l // 2
    stage_dqkv(cx, pg.inp(f"diff_w_qkv_{i}", [D, 3 * D]), qT_d, kT_d, v_d)


def emit_odd_rest(cx, pg, l, qT_d, kT_d, v_d, kTp_d, vp_d, mem_ap):
    nc = cx.nc
    i = l // 2
    ln_g = pg.inp("ln_g", [DEPTH, 3, D])
    ln_b = pg.inp("ln_b", [DEPTH, 3, D])
    with ExitStack() as les:
        AT = les.enter_context(nc.sbuf_tensor(f"dAT{l}", [128, 16, T], BF16))
        stage_datt(cx, AT, qT_d, kT_d, v_d, kTp_d, vp_d, pg.inp("rel_table", [32, 16]),
                   pg.inp(f"diff_lam_q1_{i}", [64]), pg.inp(f"diff_lam_k1_{i}", [64]),
                   pg.inp(f"diff_lam_q2_{i}", [64]), pg.inp(f"diff_lam_k2_{i}", [64]),
                   pg.inp(f"diff_subln_g_{i}", [128]), lam_init_of(l))
        stage_proj_ln(cx, "do", AT, 16, pg.inp(f"diff_w_o_{i}", [D, D]), ln_g[l, 0], ln_b[l, 0])
    emit_xattn(cx, pg, l, mem_ap)
    emit_ffn(cx, pg, l)


def build_part(kind, l):
    pg = Prog()
    nc = pg.nc
    cx = Ctx()
    cx.nc = nc
    es = ExitStack()
    SemPool(nc, es)
    cx.XT = es.enter_context(nc.sbuf_tensor("sb_XT", [128, 16, T], BF16))
    cx.ident = es.enter_context(nc.sbuf_tensor("sb_ident", [128, 128], BF16))
    cx.pflag = es.enter_context(nc.sbuf_tensor("sb_pflag", [128, 2], F32))
    cx.pcorr = es.enter_context(nc.sbuf_tensor("sb_pcorr", [128, 4, 16], F32))
    cx.relidx = es.enter_context(nc.sbuf_tensor("sb_relidx", [128, 256], F32))
    cx.nmask = es.enter_context(nc.sbuf_tensor("sb_nmask", [128, 256], F32))
    cx.neghalf = es.enter_context(nc.sbuf_tensor("sb_neghalf", [128, 1], F32))
    cx.hbuf = pg.out("h_out", [T, D])
    st = Stage(nc, "init")
    st.add("dve", MSET(cx.neghalf[:], -0.5))
    st.dma("pool", cx.ident[:], pg.inp("idn", [128, 128]))
    st.dma("sp", cx.pflag[:], pg.inp("pflag", [128, 2]))
    st.dma("sp", cx.pcorr[:], pg.inp("pcorr", [128, 4, 16]))
    st.dma("sp", cx.relidx[:], pg.inp("relidx", [128, 256]))
    st.dma("sp", cx.nmask[:], pg.inp("nmask", [128, 256]))
    h_in = pg.inp("h_in", [T, D])
    if kind == "oa":
        for t in range(NT):
            st.dma("act", cx.hbuf[t * 128:(t + 1) * 128, :], h_in[t * 128:(t + 1) * 128, :])
    st.wait_dmas("sp")
    st.close()
    i = l // 2
    ln_g = pg.inp("ln_g", [DEPTH, 3, D])
    ln_b = pg.inp("ln_b", [DEPTH, 3, D])
    if kind == "ea":
        stage_load_x(cx, h_in)
        xh = pg.inp("xh", [16, D])
        w_in = pg.inp(f"mix_w_in_{i}", [D, 3072])
        with ExitStack() as les:
            AT = les.enter_context(nc.sbuf_tensor("mAT", [128, 16, T], BF16))
            stage_mix_a(cx, AT, w_in, pg.inp(f"gmlp_ln_g_{i}", [1024]), pg.inp(f"gmlp_ln_b_{i}", [1024]),
                        pg.inp(f"gmlp_w_s_{i}", [8, 128, 128]), pg.inp(f"gmlp_b_s_{i}", [8, 128]))
            stage_mix_b(cx, AT, w_in, pg.inp(f"pool_w_{i}", [4, 256, 256]), pg.inp(f"pool_scale_{i}", [1024]), xh)
            stage_proj_ln(cx, "mixo", AT, 16, pg.inp(f"mix_w_out_{i}", [D, D]), ln_g[l, 0], ln_b[l, 0])
        emit_xattn(cx, pg, l, pg.inp("mem", [MEM, D]))
    elif kind == "ff":
        stage_load_x(cx, h_in)
        emit_ffn(cx, pg, l)
    elif kind == "qkv":
        stage_load_x(cx, h_in)
        emit_odd_qkv(cx, pg, l, pg.out("qT", [D, T], BF16), pg.out("kT", [D, T], BF16), pg.out("v", [T, D], BF16))
    elif kind == "oa":
        qT_d = pg.inp("qT", [D, T], BF16)
        kT_d = pg.inp("kT", [D, T], BF16)
        v_d = pg.inp("v", [T, D], BF16)
        kTp_d = pg.inp("kTp", [D, T], BF16)
        vp_d = pg.inp("vp", [T, D], BF16)
        with ExitStack() as les:
            AT = les.enter_context(nc.sbuf_tensor("dAT", [128, 16, T], BF16))
            stage_datt(cx, AT, qT_d, kT_d, v_d, kTp_d, vp_d, pg.inp("rel_table", [32, 16]),
                       pg.inp(f"diff_lam_q1_{i}", [64]), pg.inp(f"diff_lam_k1_{i}", [64]),
                       pg.inp(f"diff_lam_q2_{i}", [64]), pg.inp(f"diff_lam_k2_{i}", [64]),
                       pg.inp(f"diff_subln_g_{i}", [128]), lam_init_of(l))
            stage_proj_ln(cx, "do", AT, 16, pg.inp(f"diff_w_o_{i}", [D, D]), ln_g[l, 0], ln_b[l, 0])
        emit_xattn(cx, pg, l, pg.inp("mem", [MEM, D]))
    es.close()
    return pg


def _t5_bucket(rel):
    half = 16
    max_exact = 8
    ret = np.where(rel > 0, half, 0)
    n = np.abs(rel)
    nf = np.maximum(n, 1).astype(np.float32)
    large = max_exact + (np.log(nf / max_exact) / math.log(128 / max_exact) * (half - max_exact)).astype(np.int32)
    large = np.minimum(large, half - 1)
    return ret + np.where(n < max_exact, n, large)


def _consts(half):
    c = {}
    c["idn"] = np.eye(128, dtype=np.float32)
    pf = np.zeros((128, 2), np.float32)
    pf[:, 0] = 0.0 if half == 1 else NEG
    pf[:, 1] = 1.0 if half == 1 else 0.0
    c["pflag"] = pf
    pc = np.zeros((128, 4, 16), np.float32)
    for g, w in enumerate((2, 4, 8, 16)):
        for t in range(16):
            pc[:, g, t] = 1.0 / (w if half == 1 else min(t + 1, w))
    c["pcorr"] = pc
    i = np.arange(128)[:, None]
    j = np.arange(256)[None, :]
    rel = (j - 128) - i
    c["relidx"] = _t5_bucket(rel).astype(np.float32)
    allowed = ((j - 128) // 64) <= (i // 64)
    c["nmask"] = np.where(allowed, 0.0, NEG).astype(np.float32)
    return c


_PROGS = {}


def _get_prog(kind, l):
    k = (kind, l)
    if k not in _PROGS:
        _PROGS[k] = build_part(kind, l)
    return _PROGS[k]


def _run_part(pg, resolve):
    in_maps = []
    for c in range(8):
        m = {}
        for name in pg.ins:
            m[name] = np.ascontiguousarray(resolve(name, c))
        in_maps.append(m)
    res = run_bass_kernel_spmd(pg.nc, in_maps, core_ids=list(range(8)))
    return res.results


def _weight(inputs, name):
    if name in inputs:
        return inputs[name]
    if name.startswith("moe_w_routerT_"):
        i = int(name.split("_")[-1])
        return np.ascontiguousarray(inputs["moe_w_router"][i].T)
    base, rest = None, None
    for k in inputs:
        if name.startswith(k + "_") and (base is None or len(k) > len(base)):
            base, rest = k, name[len(k) + 1:]
    assert base is not None, name
    a = inputs[base]
    for tok in rest.split("_"):
        a = a[int(tok)]
    return a


def make_resolver(inputs, state, consts):
    x = inputs["x"]
    mem = inputs["mem"]
    zeros_xh = np.zeros((16, D), np.float32)

    def resolve(name, c):
        b, half = c // 2, c % 2
        if name in consts[half]:
            return consts[half][name]
        if name == "mem":
            return mem[b]
        if name == "h_in":
            return state["h"][c]
        if name == "xh":
            return state["h"][c - 1][T - 16:T] if half == 1 else zeros_xh
        if name in ("qT", "kT", "v"):
            return state[name][c]
        if name in ("kTp", "vp"):
            src = c - 1 if half == 1 else c
            return state[name[:-1]][src]
        return _weight(inputs, name)
    return resolve


SCHEDULE = [("ea", 0), ("ff", 0), ("qkv", 1), ("oa", 1), ("ff", 1), ("ea", 2), ("ff", 2), ("qkv", 3), ("oa", 3), ("ff", 3)]


def kernel(**inputs):
    x = inputs["x"]
    consts = [_consts(h) for h in (0, 1)]
    state = {"h": [x[c // 2, (c % 2) * T:(c % 2 + 1) * T] for c in range(8)]}
    resolve = make_resolver(inputs, state, consts)
    for kind, l in SCHEDULE:
        pg = _get_prog(kind, l)
        res = _run_part(pg, resolve)
        state["h"] = [res[c]["h_out"] for c in range(8)]
        if kind == "qkv":
            for nm in ("qT", "kT", "v"):
                state[nm] = [res[c][nm] for c in range(8)]
    out = np.empty((4, SEQ, D), np.float32)
    for c in range(8):
        out[c // 2, (c % 2) * T:(c % 2 + 1) * T] = state["h"][c]
    return out
```
